# Optimizing a Trainium2 kernel written in Bass

```python
import math
import jax, jax.numpy as jnp
from jax import lax
import numpy as np

D_MODEL = 2048
BATCH = 1
SEQ = 16384
DEPTH = 1

GRID_W = 64
CTX_LEN = 256
HEAD_DIM = 128
N_HEADS = 8
N_KV_HEADS = 2
GROUP = N_HEADS // N_KV_HEADS
WINDOW = 128
BLK = 128
Q_DIM = N_HEADS * HEAD_DIM
KV_DIM = N_KV_HEADS * HEAD_DIM
N_FGROUPS = 4
FG = 256
F_DIM = N_FGROUPS * FG
D_IN = Q_DIM + 2 * KV_DIM + F_DIM
D_MIX = Q_DIM + F_DIM
N_EXPERTS = 16
CAP_FACTOR = 2
EXPERT_FF = 1024
ROPE_THETA = 10000.0
RMS_EPS = 1e-6
NEG_INF = -1e30

kernel_name = "hybrid_swa_fnet_ec_moe_dit"


def _rmsnorm(x, g):
    xf = x.astype(jnp.float32)
    y = xf * lax.rsqrt(jnp.mean(xf * xf, axis=-1, keepdims=True) + RMS_EPS)
    return (y * g.astype(jnp.float32)).astype(x.dtype)


def _modulate(h, shift, scale):
    return h * (1 + scale) + shift


def _axial_rope_tables(n, dtype):
    rows = n // GRID_W
    row = jnp.repeat(jnp.arange(rows, dtype=jnp.float32), GRID_W)
    col = jnp.tile(jnp.arange(GRID_W, dtype=jnp.float32), rows)
    quarter = HEAD_DIM // 4
    freqs = ROPE_THETA ** (-jnp.arange(quarter, dtype=jnp.float32) / quarter)
    ar = row[:, None] * freqs[None, :]
    ac = col[:, None] * freqs[None, :]
    ang = jnp.concatenate([ar, ar, ac, ac], axis=-1)
    return jnp.cos(ang).astype(dtype), jnp.sin(ang).astype(dtype)


def _apply_rope(x, cos, sin):
    shp = x.shape
    xr = x.reshape(shp[:-1] + (2, 2, HEAD_DIM // 4))
    rot = jnp.stack([-xr[..., 1, :], xr[..., 0, :]], axis=-2).reshape(shp)
    return x * cos[None, :, None, :] + rot * sin[None, :, None, :]


def _softmax_with_sink(s, sink):
    s = s.astype(jnp.float32)
    sk = jnp.broadcast_to(sink.astype(jnp.float32), s.shape[:-1] + (1,))
    p = jax.nn.softmax(jnp.concatenate([s, sk], axis=-1), axis=-1)
    return p[..., :-1]


def _split_proj(p):
    q = p[..., :Q_DIM]
    k = p[..., Q_DIM:Q_DIM + KV_DIM]
    v = p[..., Q_DIM + KV_DIM:Q_DIM + 2 * KV_DIM]
    u = p[..., Q_DIM + 2 * KV_DIM:]
    b, n = p.shape[0], p.shape[1]
    return (q.reshape(b, n, N_HEADS, HEAD_DIM), k.reshape(b, n, N_KV_HEADS, HEAD_DIM),
            v.reshape(b, n, N_KV_HEADS, HEAD_DIM), u)


def _windowed_attention(q, k, v, k_ctx, v_ctx, sink):
    b, n = q.shape[0], q.shape[1]
    nb = n // BLK
    scale = 1.0 / math.sqrt(HEAD_DIM)
    qb = q.reshape(b, nb, BLK, N_KV_HEADS, GROUP, HEAD_DIM)

    def band(t):
        tb = jnp.pad(t.reshape(b, nb, BLK, N_KV_HEADS, HEAD_DIM),
                     ((0, 0), (1, 1), (0, 0), (0, 0), (0, 0)))
        return jnp.concatenate([tb[:, :-2], tb[:, 1:-1], tb[:, 2:]], axis=2)

    kw, vw = band(k), band(v)
    blk = jnp.arange(nb)[:, None, None]
    qpos = blk * BLK + jnp.arange(BLK)[None, :, None]
    kpos = blk * BLK - BLK + jnp.arange(3 * BLK)[None, None, :]
    valid = (jnp.abs(qpos - kpos) <= WINDOW) & (kpos >= 0) & (kpos < n)

    s_loc = jnp.einsum('bnqhgd,bnkhd->bhgnqk', qb, kw).astype(jnp.float32) * scale
    s_loc = jnp.where(valid, s_loc, NEG_INF)
    s_ctx = jnp.einsum('bnqhgd,bmhd->bhgnqm', qb, k_ctx).astype(jnp.float32) * scale
    sink_b = sink.reshape(N_KV_HEADS, GROUP)[None, :, :, None, None, None]
    p = _softmax_with_sink(jnp.concatenate([s_loc, s_ctx], axis=-1), sink_b).astype(v.dtype)
    p_loc, p_ctx = p[..., :3 * BLK], p[..., 3 * BLK:]
    o = (jnp.einsum('bhgnqk,bnkhd->bnqhgd', p_loc, vw)
         + jnp.einsum('bhgnqm,bmhd->bnqhgd', p_ctx, v_ctx))
    return o.reshape(b, n, Q_DIM)


def _context_attention(q, k, v, sink):
    b, m = q.shape[0], q.shape[1]
    scale = 1.0 / math.sqrt(HEAD_DIM)
    qg = q.reshape(b, m, N_KV_HEADS, GROUP, HEAD_DIM)
    s = jnp.einsum('bqhgd,bkhd->bhgqk', qg, k).astype(jnp.float32) * scale
    sink_b = sink.reshape(N_KV_HEADS, GROUP)[None, :, :, None, None]
    p = _softmax_with_sink(s, sink_b).astype(v.dtype)
    return jnp.einsum('bhgqk,bkhd->bqhgd', p, v).reshape(b, m, Q_DIM)


def _fourier_mix(u, w_f):
    b, n = u.shape[0], u.shape[1]
    ug = u.reshape(b, n, N_FGROUPS, FG).astype(jnp.float32)
    y = jnp.fft.fft2(ug, axes=(1, 3), norm='ortho').real.astype(u.dtype)
    return jnp.einsum('bngc,gcd->bngd', y, w_f).reshape(b, n, F_DIM)


def _mixer(h_x, h_c, w_in, w_out, w_f, sink, with_ctx_out):
    qx, kx, vx, ux = _split_proj(h_x @ w_in)
    qc, kc, vc, uc = _split_proj(h_c @ w_in)
    cos, sin = _axial_rope_tables(h_x.shape[1], h_x.dtype)
    qx, kx = _apply_rope(qx, cos, sin), _apply_rope(kx, cos, sin)
    ax = _windowed_attention(qx, kx, vx, kc, vc, sink)
    fx = _fourier_mix(ux, w_f)
    out_x = jnp.concatenate([ax, fx], axis=-1) @ w_out
    if not with_ctx_out:
        return out_x, None
    ac = _context_attention(qc, kc, vc, sink)
    fc = _fourier_mix(uc, w_f)
    out_c = jnp.concatenate([ac, fc], axis=-1) @ w_out
    return out_x, out_c


def _expert_choice_ffn(h, w_router, w_gate, w_up, w_down):
    b, n, d = h.shape
    cap = max(1, CAP_FACTOR * n // N_EXPERTS)
    logits = jnp.einsum('bnd,de->ben', h, w_router).astype(jnp.float32)
    aff = jax.nn.softmax(logits, axis=1)
    gates, idx = lax.top_k(aff, cap)
    xs = jax.vmap(lambda hb, ib: hb[ib])(h, idx)
    g = jnp.einsum('becd,edf->becf', xs, w_gate)
    u = jnp.einsum('becd,edf->becf', xs, w_up)
    y = jnp.einsum('becf,efd->becd', jax.nn.silu(g) * u, w_down)
    y = y * gates[..., None].astype(y.dtype)

    def combine(ib, yb):
        return jnp.zeros((n, d), yb.dtype).at[ib.reshape(-1)].add(yb.reshape(-1, d))

    return jax.vmap(combine)(idx, y)


def setup_inputs(seed: int = 0) -> dict:
    key = jax.random.key(seed)
    ks = jax.random.split(key, 20)
    f32 = jnp.float32
    L, D = DEPTH, D_MODEL
    nrm = lambda k, shp, s: jax.random.normal(k, shp, f32) * s
    return {
        'x': nrm(ks[0], (BATCH, SEQ, D), 1.0),
        'c': nrm(ks[1], (BATCH, D), 1.0),
        'ctx': nrm(ks[2], (BATCH, CTX_LEN, D), 1.0),
        'c_ctx': nrm(ks[3], (D,), 1.0),
        'w_mod': nrm(ks[4], (L, D, 6 * D), 0.5 * D ** -0.5),
        'b_mod': nrm(ks[5], (L, 6 * D), 0.02),
        'norm_mix': 1.0 + nrm(ks[6], (L, D), 0.05),
        'w_in': nrm(ks[7], (L, D, D_IN), D ** -0.5),
        'sink': nrm(ks[8], (L, N_HEADS), 0.5),
        'w_fourier': nrm(ks[9], (L, N_FGROUPS, FG, FG), FG ** -0.5),
        'w_out': nrm(ks[10], (L, D_MIX, D), D_MIX ** -0.5),
        'norm_ffn': 1.0 + nrm(ks[11], (L, D), 0.05),
        'w_router': nrm(ks[12], (L, D, N_EXPERTS), D ** -0.5),
        'w_gate': nrm(ks[13], (L, N_EXPERTS, D, EXPERT_FF), D ** -0.5),
        'w_up': nrm(ks[14], (L, N_EXPERTS, D, EXPERT_FF), D ** -0.5),
        'w_down': nrm(ks[15], (L, N_EXPERTS, EXPERT_FF, D), EXPERT_FF ** -0.5),
        'norm_final': 1.0 + nrm(ks[16], (D,), 0.05),
    }


def reference(x, c, ctx, c_ctx, w_mod, b_mod, norm_mix, w_in, sink, w_fourier, w_out,
              norm_ffn, w_router, w_gate, w_up, w_down, norm_final):
    b = x.shape[0]
    for l in range(DEPTH):
        last = l == DEPTH - 1
        mod_x = (jax.nn.silu(c) @ w_mod[l] + b_mod[l]).reshape(b, 6, 1, D_MODEL)
        mod_c = (jax.nn.silu(c_ctx) @ w_mod[l] + b_mod[l]).reshape(6, D_MODEL)

        h_x = _modulate(_rmsnorm(x, norm_mix[l]), mod_x[:, 0], mod_x[:, 1])
        h_c = _modulate(_rmsnorm(ctx, norm_mix[l]), mod_c[0], mod_c[1])
        out_x, out_c = _mixer(h_x, h_c, w_in[l], w_out[l], w_fourier[l], sink[l], not last)
        x = x + mod_x[:, 2] * out_x

        g_x = _modulate(_rmsnorm(x, norm_ffn[l]), mod_x[:, 3], mod_x[:, 4])
        x = x + mod_x[:, 5] * _expert_choice_ffn(g_x, w_router[l], w_gate[l], w_up[l], w_down[l])

        if not last:
            ctx = ctx + mod_c[2] * out_c
            g_c = _modulate(_rmsnorm(ctx, norm_ffn[l]), mod_c[3], mod_c[4])
            ctx = ctx + mod_c[5] * _expert_choice_ffn(g_c, w_router[l], w_gate[l], w_up[l], w_down[l])
    return _rmsnorm(x, norm_final)
```

```python
import math
import os
from contextlib import ExitStack
import numpy as np
import ml_dtypes
import concourse.bass as bass
import concourse.mybir as mybir
from concourse.bass_utils import run_bass_kernel_spmd

F32 = mybir.dt.float32
BF16 = mybir.dt.bfloat16
I32 = mybir.dt.int32
ALU = mybir.AluOpType
AF = mybir.ActivationFunctionType
AX = mybir.AxisListType

NCORES = 8
D = 2048
SEQ = 16384
TOK = SEQ // NCORES
NT = TOK // 128
CTX = 256
NE = 16
FF = 1024
CAP = 2 * SEQ // NE
EPS = 1e-6
CAPL = 384


class Buf:
    def __init__(self, name, multi=False):
        self.name = name
        self.multi = multi
        self.w = {}
        self.r = {}

    def reset(self):
        self.w = {}
        self.r = {}


class DSem:
    def __init__(self, nc, name):
        self.sem = nc.alloc_semaphore(name)
        self.n = 0


class Eng:
    def __init__(self, nc, raw, name, self_sync=True):
        self.raw = raw
        self.name = name
        self.sem = nc.alloc_semaphore("e_" + name)
        self.n = 0
        self.waited = {}
        self.self_sync = self_sync

    def wait(self, sem, val):
        if sem is self.sem and not self.self_sync:
            return
        if self.waited.get(sem, 0) >= val:
            return
        self.raw.wait_ge(sem, val)
        self.waited[sem] = val


class Tracker:
    def __init__(self, nc):
        self.nc = nc
        self.pe = Eng(nc, nc.tensor, "pe", self_sync=False)
        self.dve = Eng(nc, nc.vector, "dve")
        self.act = Eng(nc, nc.scalar, "act")
        self.pool = Eng(nc, nc.gpsimd, "pool")
        self.sp = Eng(nc, nc.sync, "sp")
        self.dsems = {}
        self.nins = 0

    def dsem(self, name):
        if name not in self.dsems:
            self.dsems[name] = DSem(self.nc, "d_" + name)
        return self.dsems[name]

    def _deps(self, reads, writes):
        deps = {}

        def add(d):
            for s, v in d.items():
                if deps.get(s, 0) < v:
                    deps[s] = v

        for b in reads:
            add(b.w)
        for b in writes:
            if not b.multi:
                add(b.w)
                add(b.r)
            else:
                add(b.r)
        return deps

    def _record(self, sem, val, reads, writes):
        for b in reads:
            b.r[sem] = val
        for b in writes:
            if b.multi:
                b.w[sem] = val
            else:
                b.w = {sem: val}
                b.r = {}

    def op(self, eng, emit, reads=(), writes=()):
        for s, v in self._deps(reads, writes).items():
            eng.wait(s, v)
        ins = emit(eng.raw)
        eng.n += 1
        ins.then_inc(eng.sem, 1)
        self._record(eng.sem, eng.n, reads, writes)
        self.nins += 1
        return ins

    def dma(self, eng, ds, out, in_, reads=(), writes=(), **kw):
        for s, v in self._deps(reads, writes).items():
            eng.wait(s, v)
        ins = eng.raw.dma_start(out=out, in_=in_, **kw)
        ds.n += 16
        ins.then_inc(ds.sem, 16)
        self._record(ds.sem, ds.n, reads, writes)
        self.nins += 1
        return ins

    def barrier(self, extra=()):
        engs = [self.pe, self.dve, self.act, self.pool, self.sp]
        for e in engs:
            for o in engs:
                if o is not e and o.n > 0:
                    e.wait(o.sem, o.n)
            for ds in self.dsems.values():
                if ds.n > 0:
                    e.wait(ds.sem, ds.n)
            for sem, val in extra:
                if val > 0:
                    e.wait(sem, val)

    def wait_all(self, eng, bufs):
        for b in bufs:
            for s, v in list(b.w.items()) + list(b.r.items()):
                eng.wait(s, v)


class Scope(ExitStack):
    def __init__(self, T, extra):
        super().__init__()
        self._T = T
        self._extra = extra

    def __exit__(self, *a):
        if a[0] is None:
            self._T.barrier(self._extra())
        return super().__exit__(*a)

    def close(self):
        self._T.barrier(self._extra())
        super().close()


def bc_mid(ap, n):
    return ap.unsqueeze(1).broadcast_to([ap.shape[0], n, ap.shape[1]])


def build(debug=False, stop_after=99):
    nc = bass.Bass("TRN2", target_bir_lowering=False)
    T = Tracker(nc)
    pe, dve, act, pool, sp = T.pe, T.dve, T.act, T.pool, T.sp
    RG = [list(range(NCORES))]

    def din(name, shape, dt=F32):
        return nc.dram_tensor(name, shape, dt, kind="ExternalInput")

    xh = din("xh", [2304, D])
    ctxi = din("ctx", [CTX, D])
    cci = din("cc", [128, 16, 2])
    wmod = din("wmod", [D, 1536])
    bmod = din("bmod", [1, 1536])
    nmix = din("nmix", [1, D])
    nffn = din("nffn", [1, D])
    nfin = din("nfin", [1, D])
    wqkv = din("wqkv", [D, 1536])
    wu = din("wu", [D, 128])
    sinki = din("sink", [1, 8])
    wf = din("wf", [4, 256, 256])
    wout = din("wout", [D, D])
    wr = din("wr", [D, NE])
    NED = NE if stop_after >= 7 else 1
    wg = din("wg", [NED, D, FF])
    wup = din("wup", [NED, D, FF])
    wd = din("wd", [NED, FF, D])
    identi = din("ident", [128, 128], BF16)
    identfi = din("identf", [128, 128])
    ropei = din("rope", [2304, 256])
    maski = din("masks", [128, 4, 128], BF16)
    r1i = din("r1", [128, 256], BF16)
    twi = din("tw", [128, 2, 128])
    c2si = din("c2s", [128, 3, 128], BF16)
    ccsi = din("ccs", [128, 2, 2, 256], BF16)
    bmati = din("bmat", [128, 128])
    seli = din("sel", [128, NE], BF16)
    iotai = din("iota", [128, CAPL])
    ltrii = din("ltri", [128, 128], BF16)
    outd = nc.dram_tensor("out", [TOK, D], F32, kind="ExternalOutput")

    modb = nc.dram_tensor("modb", [2, 1536], F32)
    modg = nc.dram_tensor("modg", [16, 1536], F32)
    hTs = nc.dram_tensor("hTs", [D, TOK], BF16)
    hTall = nc.dram_tensor("hTall", [NCORES * D, TOK], BF16)
    As = nc.dram_tensor("As", [128, 32768], BF16)
    Aall = nc.dram_tensor("Aall", [NCORES * 128, 32768], BF16)
    affb = nc.dram_tensor("affb", [NE, TOK], F32)
    affg = nc.dram_tensor("affg", [128, TOK], F32)
    acc = nc.dram_tensor("acc", [TOK, D], F32)

    dbg = {}

    def dbg_out(name, shape, dt=F32):
        t = nc.dram_tensor("dbg_" + name, shape, dt, kind="ExternalOutput")
        dbg[name] = t
        return t

    Bdbg = Buf("dbg", multi=True)
    Bout = Buf("out", multi=True)
    ccsem = nc.alloc_semaphore("cc")
    ccn = [0]
    ccx = lambda: [(ccsem, ccn[0])]

    def allgather(src, dst, Bsrc, Bdst):
        if os.environ.get("KNOCC"):
            T.dma(pool, T.dsem("nocc"), dst.ap()[0:src.shape[0], :], src.ap(), reads=[Bsrc], writes=[Bdst])
            return
        for s, v in T._deps([Bsrc], [Bdst]).items():
            pool.wait(s, v)
        ccn[0] += 1
        pool.raw.collective_compute("AllGather", ALU.bypass, replica_groups=RG,
                                    ins=[src.ap()], outs=[dst.ap()]).then_inc(ccsem, 1)
        T._record(ccsem, ccn[0], [Bsrc], [Bdst])

    def mod_bcast_ap(which, i):
        return bass.AP(modg, which * 1536 + i * 256, [[0, 128], [2 * 1536, 8], [1, 256]])

    def row_bcast(dt_, n=D):
        return bass.AP(dt_, 0, [[0, 128], [1, n]])

    def finish():
        T.wait_all(sp, [Bout, Bdbg])
        T.wait_all(pool, [Bout, Bdbg])
        return nc, dbg

    with Scope(T, ccx) as es0:
        ident = es0.enter_context(nc.sbuf_tensor("s_ident", [128, 128], BF16))
        identf = es0.enter_context(nc.sbuf_tensor("s_identf", [128, 128], F32))
        ones = es0.enter_context(nc.sbuf_tensor("s_ones", [128, 128], BF16))
        esm = Scope(T, ccx)
        axT = esm.enter_context(nc.sbuf_tensor("s_axT", [128, 8, TOK], BF16))
        esq = Scope(T, ccx)
        qT = esq.enter_context(nc.sbuf_tensor("s_qT", [128, 8, TOK], BF16))
        kT = esq.enter_context(nc.sbuf_tensor("s_kT", [128, 2, 2560], BF16))
        Vt = esq.enter_context(nc.sbuf_tensor("s_Vt", [128, 20, 256], BF16))
        Bc = Buf("consts", multi=True)
        T.dma(sp, T.dsem("c0"), ident[:], identi.ap(), writes=[Bc])
        T.dma(sp, T.dsem("c0"), identf[:], identfi.ap(), writes=[Bc])
        T.op(pool, lambda e: e.memset(ones[:], 1.0), writes=[Bc])
        BqT, BkT, BV = Buf("qT", True), Buf("kT", True), Buf("V", True)
        Bmodg = Buf("modg")
        BhTs = Buf("hTs", True)
        BhTall = Buf("hTall")

        with Scope(T, ccx) as es:
            cct = es.enter_context(nc.sbuf_tensor("s_cct", [128, 16, 2], F32))
            sct = es.enter_context(nc.sbuf_tensor("s_sct", [128, 16, 2], F32))
            wmt = es.enter_context(nc.sbuf_tensor("s_wmt", [128, 3, 1536], F32))
            bmt = es.enter_context(nc.sbuf_tensor("s_bmt", [2, 1536], F32))
            modv = es.enter_context(nc.sbuf_tensor("s_modv", [2, 1536], F32))
            pm = es.enter_context(nc.psum_tensor("p_pm", [128, 3, 512], F32))
            Bcc, Bsc, Bbm, Bmv, Bpm, Bmodb = Buf("cc"), Buf("sc"), Buf("bm"), Buf("mv"), Buf("pm"), Buf("modb")
            Bwm = [Buf("wm%d" % i) for i in range(3)]
            T.dma(sp, T.dsem("cc"), cct[:], cci.ap(), writes=[Bcc])
            T.dma(sp, T.dsem("bm"), bmt[:], bass.AP(bmod, 0, [[0, 2], [1, 1536]]), writes=[Bbm])
            T.op(act, lambda e: e.activation(out=sct[:], in_=cct[:], func=AF.Silu), reads=[Bcc], writes=[Bsc])
            for kc in range(16):
                s = kc % 3
                T.dma(sp, T.dsem("wm%d" % s), wmt[:, s, :], wmod.ap()[kc * 128:(kc + 1) * 128, :], writes=[Bwm[s]])
                for n in range(3):
                    T.op(pe, lambda e: e.matmul(pm[0:2, n, :], lhsT=sct[:, kc, :], rhs=wmt[:, s, n * 512:(n + 1) * 512],
                                                start=(kc == 0), stop=(kc == 15)),
                         reads=[Bsc, Bwm[s]], writes=[Bpm])
            T.op(dve, lambda e: e.tensor_tensor(out=modv[:], in0=pm[0:2].rearrange("p a b -> p (a b)"), in1=bmt[:], op=ALU.add),
                 reads=[Bpm, Bbm], writes=[Bmv])
            T.dma(sp, T.dsem("mv"), modb.ap(), modv[:], reads=[Bmv], writes=[Bmodb])
            allgather(modb, modg, Bmodb, Bmodg)
            if debug:
                d = dbg_out("modg", [16, 1536])
                T.dma(pool, T.dsem("dbg"), d.ap(), modg.ap(), reads=[Bmodg], writes=[Bdbg])
        if stop_after <= 0:
            esq.close()
            return finish()

        with Scope(T, ccx) as es:
            wq = es.enter_context(nc.sbuf_tensor("s_wq", [128, 16, 1536], BF16))
            a1 = es.enter_context(nc.sbuf_tensor("s_a1", [128, D], F32))
            b1 = es.enter_context(nc.sbuf_tensor("s_b1", [128, D], F32))
            xt = es.enter_context(nc.sbuf_tensor("s_xt", [128, 2, D], F32))
            tmp = es.enter_context(nc.sbuf_tensor("s_tmp", [128, D], F32))
            hb = es.enter_context(nc.sbuf_tensor("s_hb", [128, 2, D], BF16))
            hT = es.enter_context(nc.sbuf_tensor("s_hT", [128, 2, 16, 128], BF16))
            cs = es.enter_context(nc.sbuf_tensor("s_cs", [128, 2, 256], F32))
            t1 = es.enter_context(nc.sbuf_tensor("s_t1", [128, 1280], F32))
            t2 = es.enter_context(nc.sbuf_tensor("s_t2", [128, 1280], F32))
            qkb = es.enter_context(nc.sbuf_tensor("s_qkb", [128, 1280], BF16))
            st1 = es.enter_context(nc.sbuf_tensor("s_st1", [128, 20, 4], F32))
            pth = es.enter_context(nc.psum_tensor("p_pth", [128, 2, 1024], BF16))
            pq = es.enter_context(nc.psum_tensor("p_pq", [128, 1536], F32))
            ptq = es.enter_context(nc.psum_tensor("p_ptq", [128, 10, 128], BF16))
            Bwq, Ba1, Bb1, Btmp = Buf("wq", True), Buf("a1"), Buf("b1"), Buf("tmp")
            Bxt = [Buf("xt0"), Buf("xt1")]
            Bhb = [Buf("hb0"), Buf("hb1")]
            BhT = [Buf("hT0"), Buf("hT1")]
            Bcs = [Buf("cs0"), Buf("cs1")]
            Bt1, Bt2, Bqkb, Bpth, Bpq, Bptq = Buf("t1"), Buf("t2"), Buf("qkb"), Buf("pth"), Buf("pq"), Buf("ptq")
            wqv = wqkv.ap().rearrange("(kc p) n -> p kc n", p=128)
            for kc in range(16):
                T.dma(pool, T.dsem("wq"), wq[:, kc, :], wqv[:, kc, :], writes=[Bwq])
            T.op(pool, lambda e: e.memset(st1[:], 0.0), writes=[Bc])

            def load_mod1(which):
                T.dma(sp, T.dsem("a1"), a1[:].rearrange("p (r q) -> p r q", r=8), mod_bcast_ap(which, 1), reads=[Bmodg], writes=[Ba1])
                T.dma(sp, T.dsem("b1"), b1[:].rearrange("p (r q) -> p r q", r=8), mod_bcast_ap(which, 0), reads=[Bmodg], writes=[Bb1])
                T.dma(sp, T.dsem("tmp"), tmp[:], row_bcast(nmix), writes=[Btmp])
                T.op(dve, lambda e: e.scalar_tensor_tensor(out=a1[:], in0=a1[:], scalar=1.0, in1=tmp[:], op0=ALU.add, op1=ALU.mult),
                     reads=[Btmp], writes=[Ba1])

            jobs = [("ctx", c) for c in range(2)] + [("x", i) for i in range(18)]
            jobs = jobs[:int(os.environ.get("KJOBS", "20"))]
            KLVL = int(os.environ.get("KLVL", "9"))

            def src_ap(job):
                kind, i = job
                if kind == "ctx":
                    return ctxi.ap()[i * 128:(i + 1) * 128, :]
                return xh.ap()[i * 128:(i + 1) * 128, :]

            def load_x(ji):
                b = ji % 2
                T.dma(sp, T.dsem("xt%d" % b), xt[:, b, :], src_ap(jobs[ji]), writes=[Bxt[b]])
                kind, i = jobs[ji]
                if kind == "x":
                    T.dma(sp, T.dsem("cs%d" % b), cs[:, b, :], ropei.ap()[i * 128:(i + 1) * 128, :], writes=[Bcs[b]])

            load_mod1(1)
            load_x(0)
            for ji, (kind, i) in enumerate(jobs):
                b = ji % 2
                if ji == 2:
                    load_mod1(0)
                if ji + 1 < len(jobs):
                    load_x(ji + 1)
                local = kind == "x" and 1 <= i <= 16
                Bst = Buf("st")
                sc = st1[:, ji, :]
                T.op(act, lambda e: e.activation(out=tmp[:], in_=xt[:, b, :], func=AF.Square, accum_out=sc[:, 0:1]),
                     reads=[Bxt[b], Bc], writes=[Btmp, Bst])
                T.op(dve, lambda e: e.tensor_scalar(out=sc[:, 1:2], in0=sc[:, 0:1], scalar1=1.0 / D, scalar2=EPS, op0=ALU.mult, op1=ALU.add),
                     reads=[Bst], writes=[Bst])
                T.op(act, lambda e: e.activation(out=sc[:, 2:3], in_=sc[:, 1:2], func=AF.Sqrt), reads=[Bst], writes=[Bst])
                T.op(dve, lambda e: e.reciprocal(out=sc[:, 3:4], in_=sc[:, 2:3]), reads=[Bst], writes=[Bst])
                T.op(dve, lambda e: e.scalar_tensor_tensor(out=tmp[:], in0=xt[:, b, :], scalar=sc[:, 3:4], in1=a1[:], op0=ALU.mult, op1=ALU.mult),
                     reads=[Bxt[b], Bst, Ba1], writes=[Btmp])
                T.op(pool, lambda e: e.tensor_tensor(out=hb[:, b, :], in0=tmp[:], in1=b1[:], op=ALU.add),
                     reads=[Btmp, Bb1], writes=[Bhb[b]])
                if KLVL <= 1:
                    continue
                for kc in range(16):
                    T.op(pe, lambda e: e.transpose(pth[:, kc // 8, (kc % 8) * 128:(kc % 8 + 1) * 128], hb[:, b, kc * 128:(kc + 1) * 128], ident[:]),
                         reads=[Bhb[b], Bc], writes=[Bpth])
                for hh in range(2):
                    T.op(act, lambda e: e.activation(out=hT[:, b, hh * 8:(hh + 1) * 8, :], in_=pth[:, hh, :].rearrange("p (a c) -> p a c", a=8), func=AF.Copy),
                         reads=[Bpth], writes=[BhT[b]])
                if KLVL <= 2:
                    continue
                if local:
                    T.dma(sp, T.dsem("hT%d" % b), hTs.ap().rearrange("(kc p) t -> p kc t", p=128)[:, :, (i - 1) * 128:i * 128], hT[:, b, :, :],
                          reads=[BhT[b]], writes=[BhTs])
                chunks = [0, 1, 2] if local else [2]
                for n in chunks:
                    for kc in range(16):
                        T.op(pe, lambda e: e.matmul(pq[:, n * 512:(n + 1) * 512], lhsT=hT[:, b, kc, :], rhs=wq[:, kc, n * 512:(n + 1) * 512],
                                                    start=(kc == 0), stop=(kc == 15)),
                             reads=[BhT[b], Bwq], writes=[Bpq])
                if KLVL <= 3:
                    continue
                if kind == "ctx":
                    blk = 18 + i
                    kcol = 2304 + i * 128
                    h0, h1 = 8, 10
                    T.op(dve, lambda e: e.tensor_copy(out=qkb[:, 1024:1280], in_=pq[:, 1024:1280]), reads=[Bpq], writes=[Bqkb])
                else:
                    blk = i
                    kcol = i * 128
                    h0, h1 = (0, 10) if local else (8, 10)
                    H = h1 - h0
                    cosb = bc_mid(cs[:, b, 0:128], H)
                    pqv = pq[:, h0 * 128:h1 * 128].rearrange("p (h a c f) -> p h a c f", h=H, a=2, c=2)
                    t2v = t2[:, h0 * 128:h1 * 128].rearrange("p (h a c f) -> p h a c f", h=H, a=2, c=2)
                    sinv = cs[:, b, 128:256].rearrange("p (a c f) -> p a c f", a=2, c=2)
                    T.op(dve, lambda e: e.tensor_tensor(out=t1[:, h0 * 128:h1 * 128].rearrange("p (h f) -> p h f", h=H),
                                                        in0=pq[:, h0 * 128:h1 * 128].rearrange("p (h f) -> p h f", h=H), in1=cosb, op=ALU.mult),
                         reads=[Bpq, Bcs[b]], writes=[Bt1])
                    for c_ in range(2):
                        sb_ = sinv[:, :, c_, :].unsqueeze(1).broadcast_to([128, H, 2, 32])
                        T.op(dve, lambda e: e.tensor_tensor(out=t2v[:, :, :, c_, :], in0=pqv[:, :, :, 1 - c_, :], in1=sb_, op=ALU.mult),
                             reads=[Bpq, Bcs[b]], writes=[Bt2])
                    T.op(pool, lambda e: e.tensor_tensor(out=qkb[:, h0 * 128:h1 * 128], in0=t1[:, h0 * 128:h1 * 128], in1=t2[:, h0 * 128:h1 * 128], op=ALU.add),
                         reads=[Bt1, Bt2], writes=[Bqkb])
                if KLVL <= 4:
                    continue
                T.op(dve, lambda e: e.tensor_copy(out=Vt[:, blk, :], in_=pq[:, 1280:1536]), reads=[Bpq], writes=[BV])
                if KLVL <= 5:
                    continue
                for h in range(h0, h1):
                    T.op(pe, lambda e: e.transpose(ptq[:, h, :], qkb[:, h * 128:(h + 1) * 128], ident[:]), reads=[Bqkb, Bc], writes=[Bptq])
                if h0 == 0:
                    T.op(act, lambda e: e.activation(out=qT[:, :, (i - 1) * 128:i * 128], in_=ptq[:, 0:8, :], func=AF.Copy), reads=[Bptq], writes=[BqT])
                T.op(act, lambda e: e.activation(out=kT[:, :, kcol:kcol + 128], in_=ptq[:, 8:10, :], func=AF.Copy), reads=[Bptq], writes=[BkT])
            allgather(hTs, hTall, BhTs, BhTall)
            if debug:
                for nm, tt, shp, bb in (("qT", qT, [128, 8 * TOK], BqT), ("kT", kT, [128, 2 * 2560], BkT), ("V", Vt, [128, 20 * 256], BV)):
                    d = dbg_out(nm, shp, BF16)
                    T.dma(sp, T.dsem("dbg2"), d.ap(), tt[:].rearrange("p a b -> p (a b)"), reads=[bb], writes=[Bdbg])
        if stop_after <= 1:
            esq.close()
            return finish()

        BaxT, BfxT = Buf("axT", True), Buf("fxT", True)
        with Scope(T, ccx) as es2:
            mk = es2.enter_context(nc.sbuf_tensor("s_mk", [128, 4, 128], BF16))
            snk = es2.enter_context(nc.sbuf_tensor("s_snk", [1, 8], F32))
            esk = es2.enter_context(nc.sbuf_tensor("s_esk", [1, 8], F32))
            esr = es2.enter_context(nc.sbuf_tensor("s_esr", [1, 8, 128], BF16))
            PT = es2.enter_context(nc.sbuf_tensor("s_PT", [128, 2, 5, 512], BF16))
            rc = es2.enter_context(nc.sbuf_tensor("s_rc", [128, 512], F32))
            Sp = es2.enter_context(nc.psum_tensor("p_S", [128, 5, 512], F32))
            OTp = es2.enter_context(nc.psum_tensor("p_OT", [128, 512], F32))
            DNp = es2.enter_context(nc.psum_tensor("p_DN", [128, 512], F32))
            Bmk, Bsnk, Besr, Brc, BSp, BOT, BDN = Buf("mk"), Buf("snk"), Buf("esr"), Buf("rc"), Buf("Sp"), Buf("OT"), Buf("DN")
            BPT = [Buf("PT0"), Buf("PT1")]
            T.dma(sp, T.dsem("mk"), mk[:], maski.ap(), writes=[Bmk])
            T.dma(sp, T.dsem("snk"), snk[:], sinki.ap(), writes=[Bsnk])
            T.op(act, lambda e: e.activation(out=esk[:], in_=snk[:], func=AF.Exp), reads=[Bsnk], writes=[Bsnk])
            T.op(dve, lambda e: e.tensor_copy(out=esr[:], in_=esk[:].unsqueeze(2).broadcast_to([1, 8, 128])), reads=[Bsnk], writes=[Besr])
            scale = 1.0 / math.sqrt(128.0)
            it = 0
            for blk in range(NT):
                for kvh in range(2):
                    pb = it % 2
                    it += 1
                    kcols = [blk * 128, (blk + 1) * 128, (blk + 2) * 128, 2304, 2432]
                    vblk = [blk, blk + 1, blk + 2, 18, 19]
                    qv = qT[:, 4 * kvh:4 * kvh + 4, blk * 128:(blk + 1) * 128]
                    for j in range(5):
                        T.op(pe, lambda e: e.matmul(Sp[:, j, :].rearrange("p (h q) -> p h q", h=4), lhsT=kT[:, kvh, kcols[j]:kcols[j] + 128], rhs=qv, start=True, stop=True),
                             reads=[BkT, BqT], writes=[BSp])
                    for j in range(5):
                        T.op(act, lambda e: e.activation(out=PT[:, pb, j, :], in_=Sp[:, j, :], func=AF.Exp, scale=scale), reads=[BSp], writes=[BPT[pb]])
                    mp = 0 if blk == 0 else 1
                    mn = 2 if blk == NT - 1 else 3
                    for j, mi in ((0, mp), (2, mn)):
                        pv = PT[:, pb, j, :].rearrange("p (h q) -> p h q", h=4)
                        T.op(dve, lambda e: e.tensor_tensor(out=pv, in0=pv, in1=bc_mid(mk[:, mi, :], 4), op=ALU.mult), reads=[Bmk], writes=[BPT[pb]])
                    for j in range(5):
                        T.op(pe, lambda e: e.matmul(OTp[:], lhsT=Vt[:, vblk[j], kvh * 128:(kvh + 1) * 128], rhs=PT[:, pb, j, :], start=(j == 0), stop=(j == 4)),
                             reads=[BV, BPT[pb]], writes=[BOT])
                    for j in range(5):
                        T.op(pe, lambda e: e.matmul(DNp[:], lhsT=ones[:], rhs=PT[:, pb, j, :], start=(j == 0), stop=False),
                             reads=[Bc, BPT[pb]], writes=[BDN])
                    T.op(pe, lambda e: e.matmul(DNp[:], lhsT=ones[0:1, :], rhs=esr[0:1, 4 * kvh:4 * kvh + 4, :].rearrange("p h q -> p (h q)"), start=False, stop=True),
                         reads=[Bc, Besr], writes=[BDN])
                    T.op(dve, lambda e: e.reciprocal(out=rc[:], in_=DNp[:]), reads=[BDN], writes=[Brc])
                    T.op(dve, lambda e: e.tensor_tensor(out=axT[:, 4 * kvh:4 * kvh + 4, blk * 128:(blk + 1) * 128], in0=OTp[:].rearrange("p (h q) -> p h q", h=4),
                                                        in1=rc[:].rearrange("p (h q) -> p h q", h=4), op=ALU.mult),
                         reads=[BOT, Brc], writes=[BaxT])
            if debug:
                d = dbg_out("axT", [128, 8 * TOK], BF16)
                T.dma(sp, T.dsem("dbg2"), d.ap(), axT[:].rearrange("p a b -> p (a b)"), reads=[BaxT], writes=[Bdbg])
        esq.close()
        fxT = esm.enter_context(nc.sbuf_tensor("s_fxT", [128, 8, TOK], BF16))
        if stop_after <= 2:
            return finish()

        BAs, BAall = Buf("As", True), Buf("Aall")
        with Scope(T, ccx) as es3:
            X1 = es3.enter_context(nc.sbuf_tensor("s_X1", [128, 128, 128], BF16))
            BX1 = Buf("X1", True)
            with Scope(T, ccx) as es3a:
                UT = es3a.enter_context(nc.sbuf_tensor("s_UT", [128, SEQ], BF16))
                BUT = Buf("UT", True)
                with Scope(T, ccx) as es3b:
                    wub = es3b.enter_context(nc.sbuf_tensor("s_wub", [128, 16, 128], BF16))
                    hTc = es3b.enter_context(nc.sbuf_tensor("s_hTc", [128, 2, 16, 512], BF16))
                    pu = es3b.enter_context(nc.psum_tensor("p_u", [128, 2, 512], F32))
                    Bwub = Buf("wub")
                    BhTc = [Buf("hTc0"), Buf("hTc1")]
                    Bpu = [Buf("pu0"), Buf("pu1")]
                    T.dma(pool, T.dsem("wub"), wub[:], wu.ap().rearrange("(kc p) n -> p kc n", p=128), writes=[Bwub])
                    hv = hTall.ap().rearrange("(r kc p) t -> r p kc t", r=NCORES, kc=16)

                    def load_h(tc):
                        b = tc % 2
                        T.dma(sp, T.dsem("hTc%d" % b), hTc[:, b, :, :], hv[tc // 4][:, :, (tc % 4) * 512:(tc % 4 + 1) * 512], reads=[BhTall], writes=[BhTc[b]])

                    load_h(0)
                    for tc in range(32):
                        b = tc % 2
                        if tc + 1 < 32:
                            load_h(tc + 1)
                        for kc in range(16):
                            T.op(pe, lambda e: e.matmul(pu[:, b, :], lhsT=wub[:, kc, :], rhs=hTc[:, b, kc, :], start=(kc == 0), stop=(kc == 15)),
                                 reads=[Bwub, BhTc[b]], writes=[Bpu[b]])
                        T.op(act if b == 0 else dve, (lambda e: e.activation(out=UT[:, tc * 512:(tc + 1) * 512], in_=pu[:, b, :], func=AF.Copy)) if b == 0 else
                             (lambda e: e.tensor_copy(out=UT[:, tc * 512:(tc + 1) * 512], in_=pu[:, b, :])), reads=[Bpu[b]], writes=[BUT])
                with Scope(T, ccx) as es3c:
                    ptx = es3c.enter_context(nc.psum_tensor("p_tx", [128, 2, 8, 128], BF16))
                    Bptx = [Buf("ptx0"), Buf("ptx1")]
                    UTv = UT[:].rearrange("p (a b) -> p b a", b=128)
                    for g8 in range(16):
                        pb = g8 % 2
                        for q in range(8):
                            b_ = g8 * 8 + q
                            T.op(pe, lambda e: e.transpose(ptx[:, pb, q, :], UTv[:, b_, :], ident[:]), reads=[BUT, Bc], writes=[Bptx[pb]])
                        T.op(act if pb == 0 else dve, (lambda e: e.activation(out=X1[:, g8 * 8:(g8 + 1) * 8, :], in_=ptx[:, pb, :, :], func=AF.Copy)) if pb == 0 else
                             (lambda e: e.tensor_copy(out=X1[:, g8 * 8:(g8 + 1) * 8, :], in_=ptx[:, pb, :, :])), reads=[Bptx[pb]], writes=[BX1])
            with Scope(T, ccx) as es3d:
                r1 = es3d.enter_context(nc.sbuf_tensor("s_r1", [128, 256], BF16))
                tw = es3d.enter_context(nc.sbuf_tensor("s_tw", [128, 2, 128], F32))
                c2s = es3d.enter_context(nc.sbuf_tensor("s_c2s", [128, 3, 128], BF16))
                Zs = es3d.enter_context(nc.sbuf_tensor("s_Zs", [128, 8, 2, 128], F32))
                Pa = es3d.enter_context(nc.sbuf_tensor("s_Pa", [128, 8, 2, 128], F32))
                Pb = es3d.enter_context(nc.sbuf_tensor("s_Pb", [128, 8, 2, 128], F32))
                Zg = es3d.enter_context(nc.sbuf_tensor("s_Zg", [128, 32, 2, 128], BF16))
                Ag = es3d.enter_context(nc.sbuf_tensor("s_Ag", [128, 2, 32, 128], BF16))
                Zp = es3d.enter_context(nc.psum_tensor("p_Z", [128, 8, 256], F32))
                XRp = es3d.enter_context(nc.psum_tensor("p_XR", [128, 512], F32))
                XIp = es3d.enter_context(nc.psum_tensor("p_XI", [128, 512], F32))
                Bk3, BZs, BPa, BPb, BZg, BAg, BZp, BXR, BXI = Buf("k3", True), Buf("Zs"), Buf("Pa"), Buf("Pb"), Buf("Zg"), Buf("Ag"), Buf("Zp"), Buf("XR"), Buf("XI")
                T.dma(sp, T.dsem("k3"), r1[:], r1i.ap(), writes=[Bk3])
                T.dma(sp, T.dsem("k3"), tw[:], twi.ap(), writes=[Bk3])
                T.dma(sp, T.dsem("k3"), c2s[:], c2si.ap(), writes=[Bk3])
                Asv = As.ap().rearrange("c (ri ch d) -> c ri ch d", ri=2, ch=128)
                trb = tw[:, 0, :].unsqueeze(1).unsqueeze(1).broadcast_to([128, 8, 2, 128])
                tib = tw[:, 1, :].unsqueeze(1).unsqueeze(1).broadcast_to([128, 8, 2, 128])
                for G in range(4):
                    for sub in range(4):
                        for c8 in range(8):
                            ch = G * 32 + sub * 8 + c8
                            T.op(pe, lambda e: e.matmul(Zp[:, c8, :], lhsT=X1[:, :, ch], rhs=r1[:], start=True, stop=True), reads=[BX1, Bk3], writes=[BZp])
                        T.op(act, lambda e: e.activation(out=Zs[:].rearrange("p c r d -> p (c r d)"), in_=Zp[:].rearrange("p c x -> p (c x)"), func=AF.Copy), reads=[BZp], writes=[BZs])
                        T.op(dve, lambda e: e.tensor_tensor(out=Pa[:], in0=Zs[:], in1=trb, op=ALU.mult), reads=[BZs, Bk3], writes=[BPa])
                        T.op(pool, lambda e: e.tensor_tensor(out=Pb[:], in0=Zs[:], in1=tib, op=ALU.mult), reads=[BZs, Bk3], writes=[BPb])
                        T.op(dve, lambda e: e.tensor_tensor(out=Zg[:, sub * 8:(sub + 1) * 8, 0, :], in0=Pa[:, :, 0, :], in1=Pb[:, :, 1, :], op=ALU.subtract), reads=[BPa, BPb], writes=[BZg])
                        T.op(pool, lambda e: e.tensor_tensor(out=Zg[:, sub * 8:(sub + 1) * 8, 1, :], in0=Pb[:, :, 0, :], in1=Pa[:, :, 1, :], op=ALU.add), reads=[BPa, BPb], writes=[BZg])
                    for q in range(8):
                        zr = Zg[:, q * 4:(q + 1) * 4, 0, :]
                        zi = Zg[:, q * 4:(q + 1) * 4, 1, :]
                        xr = XRp[:].rearrange("p (c d) -> p c d", c=4)
                        xi = XIp[:].rearrange("p (c d) -> p c d", c=4)
                        T.op(pe, lambda e: e.matmul(xr, lhsT=c2s[:, 0, :], rhs=zr, start=True, stop=False), reads=[BZg, Bk3], writes=[BXR])
                        T.op(pe, lambda e: e.matmul(xr, lhsT=c2s[:, 1, :], rhs=zi, start=False, stop=True), reads=[BZg, Bk3], writes=[BXR])
                        T.op(pe, lambda e: e.matmul(xi, lhsT=c2s[:, 0, :], rhs=zi, start=True, stop=False), reads=[BZg, Bk3], writes=[BXI])
                        T.op(pe, lambda e: e.matmul(xi, lhsT=c2s[:, 2, :], rhs=zr, start=False, stop=True), reads=[BZg, Bk3], writes=[BXI])
                        T.op(act, lambda e: e.activation(out=Ag[:, 0, q * 4:(q + 1) * 4, :], in_=xr, func=AF.Copy), reads=[BXR], writes=[BAg])
                        T.op(dve, lambda e: e.tensor_copy(out=Ag[:, 1, q * 4:(q + 1) * 4, :], in_=xi), reads=[BXI], writes=[BAg])
                    T.dma(sp, T.dsem("Ag"), Asv[:, :, G * 32:(G + 1) * 32, :], Ag[:], reads=[BAg], writes=[BAs])
                if debug:
                    d = dbg_out("X1", [128, 16384], BF16)
                    T.dma(sp, T.dsem("dbg2"), d.ap(), X1[:].rearrange("p a b -> p (a b)"), reads=[BX1], writes=[Bdbg])
                    d = dbg_out("Zg", [128, 8192], BF16)
                    T.dma(sp, T.dsem("dbg2"), d.ap(), Zg[:].rearrange("p a b c -> p (a b c)"), reads=[BZg], writes=[Bdbg])
                    d = dbg_out("Zs", [128, 2048])
                    T.dma(sp, T.dsem("dbg2"), d.ap(), Zs[:].rearrange("p a b c -> p (a b c)"), reads=[BZs], writes=[Bdbg])
                    d = dbg_out("Pa", [128, 2048])
                    T.dma(sp, T.dsem("dbg2"), d.ap(), Pa[:].rearrange("p a b c -> p (a b c)"), reads=[BPa], writes=[Bdbg])
                    d = dbg_out("Pb", [128, 2048])
                    T.dma(sp, T.dsem("dbg2"), d.ap(), Pb[:].rearrange("p a b c -> p (a b c)"), reads=[BPb], writes=[Bdbg])
            allgather(As, Aall, BAs, BAall)
        with Scope(T, ccx) as es3e:
            wfb = es3e.enter_context(nc.sbuf_tensor("s_wfb", [128, 4, 2, 256], BF16))
            ccs = es3e.enter_context(nc.sbuf_tensor("s_ccs", [128, 2, 2, 256], BF16))
            Mg = es3e.enter_context(nc.sbuf_tensor("s_Mg", [128, 4, 4, 256], BF16))
            Tt = es3e.enter_context(nc.sbuf_tensor("s_Tt", [128, 2, 4, TOK], BF16))
            pM = es3e.enter_context(nc.psum_tensor("p_M", [128, 256], F32))
            pf = es3e.enter_context(nc.psum_tensor("p_f", [128, 2, 512], F32))
            Bwfb, Bccs, BMg, BpM = Buf("wfb"), Buf("ccs"), Buf("Mg", True), Buf("pM")
            BTt = [Buf("Tt0", True), Buf("Tt1", True)]
            Bpf = [Buf("pf0"), Buf("pf1")]
            T.dma(pool, T.dsem("wfb"), wfb[:], wf.ap().rearrange("g (cc p) f -> p g cc f", p=128), writes=[Bwfb])
            T.dma(sp, T.dsem("ccs"), ccs[:], ccsi.ap(), writes=[Bccs])
            for g in range(4):
                for ri in range(2):
                    for half in range(2):
                        for c2c in range(2):
                            T.op(pe, lambda e: e.matmul(pM[:], lhsT=ccs[:, c2c, ri, half * 128:(half + 1) * 128], rhs=wfb[:, g, c2c, :], start=(c2c == 0), stop=(c2c == 1)),
                                 reads=[Bccs, Bwfb], writes=[BpM])
                        T.op(dve, lambda e: e.tensor_copy(out=Mg[:, g, ri * 2 + half, :], in_=pM[:]), reads=[BpM], writes=[BMg])
            pid = nc.gpsimd.partition_id()
            Av = Aall.ap().rearrange("(r c) (ri ch d) -> r ri ch c d", r=NCORES, ri=2, ch=128)
            cnt = 0
            for g in range(4):
                tb = g % 2
                for ri in range(2):
                    for half in range(2):
                        src = Av[2 * g + half, ri][:, bass.ds(pid * 16, 16), :]
                        T.dma(pool, T.dsem("Tt%d" % tb), Tt[:, tb, ri * 2 + half, :].rearrange("p (c d) -> p c d", c=16), src, reads=[BAall], writes=[BTt[tb]])
                for m in range(2):
                    for tq in range(4):
                        pb = cnt % 2
                        cnt += 1
                        for k4 in range(4):
                            T.op(pe, lambda e: e.matmul(pf[:, pb, :], lhsT=Mg[:, g, k4, m * 128:(m + 1) * 128], rhs=Tt[:, tb, k4, tq * 512:(tq + 1) * 512], start=(k4 == 0), stop=(k4 == 3)),
                                 reads=[BMg, BTt[tb]], writes=[Bpf[pb]])
                        T.op(act if pb == 0 else dve, (lambda e: e.activation(out=fxT[:, 2 * g + m, tq * 512:(tq + 1) * 512], in_=pf[:, pb, :], func=AF.Copy)) if pb == 0 else
                             (lambda e: e.tensor_copy(out=fxT[:, 2 * g + m, tq * 512:(tq + 1) * 512], in_=pf[:, pb, :])), reads=[Bpf[pb]], writes=[BfxT])
            if debug:
                d = dbg_out("Mg", [128, 4096], BF16)
                T.dma(sp, T.dsem("dbg2"), d.ap(), Mg[:].rearrange("p a b c -> p (a b c)"), reads=[BMg], writes=[Bdbg])
                d = dbg_out("As", [128, 32768], BF16)
                T.dma(sp, T.dsem("dbg2"), d.ap(), As.ap(), reads=[BAs], writes=[Bdbg])
                d = dbg_out("Tt", [128, 4 * TOK], BF16)
                T.dma(sp, T.dsem("dbg2"), d.ap(), Tt[:, 1, :, :].rearrange("p a b -> p (a b)"), reads=[BTt[1]], writes=[Bdbg])
                d = dbg_out("fxT", [128, 8 * TOK], BF16)
                T.dma(sp, T.dsem("dbg2"), d.ap(), fxT[:].rearrange("p a b -> p (a b)"), reads=[BfxT], writes=[Bdbg])
        if stop_after <= 3:
            return finish()

        def rms_stats(sc, src, Bsrc, Bst, Btm, tm):
            T.op(act, lambda e: e.activation(out=tm, in_=src, func=AF.Square, accum_out=sc[:, 0:1]), reads=[Bsrc, Bc], writes=[Btm, Bst])
            T.op(dve, lambda e: e.tensor_scalar(out=sc[:, 1:2], in0=sc[:, 0:1], scalar1=1.0 / D, scalar2=EPS, op0=ALU.mult, op1=ALU.add), reads=[Bst], writes=[Bst])
            T.op(act, lambda e: e.activation(out=sc[:, 2:3], in_=sc[:, 1:2], func=AF.Sqrt), reads=[Bst], writes=[Bst])
            T.op(dve, lambda e: e.reciprocal(out=sc[:, 3:4], in_=sc[:, 2:3]), reads=[Bst], writes=[Bst])

        BaccT = [Buf("acc%d" % t) for t in range(NT)]
        with Scope(T, ccx) as es4:
            wob = es4.enter_context(nc.sbuf_tensor("s_wob", [128, 16, D], BF16))
            g1 = es4.enter_context(nc.sbuf_tensor("s_g1", [128, D], F32))
            xt4 = es4.enter_context(nc.sbuf_tensor("s_xt4", [128, 2, D], F32))
            tm4 = es4.enter_context(nc.sbuf_tensor("s_tm4", [128, 2, D], F32))
            po = es4.enter_context(nc.psum_tensor("p_o", [128, 2, 4, 512], F32))
            Bwob, Bg1 = Buf("wob", True), Buf("g1")
            Bxt4 = [Buf("xt40"), Buf("xt41")]
            Btm4 = [Buf("tm40"), Buf("tm41")]
            Bpo = [Buf("po0"), Buf("po1")]
            wov = wout.ap().rearrange("(kc p) n -> p kc n", p=128)
            for kc in range(16):
                T.dma(pool, T.dsem("wob"), wob[:, kc, :], wov[:, kc, :], writes=[Bwob])
            T.dma(sp, T.dsem("g1"), g1[:].rearrange("p (r q) -> p r q", r=8), mod_bcast_ap(0, 2), reads=[Bmodg], writes=[Bg1])
            for t in range(NT):
                b = t % 2
                T.dma(sp, T.dsem("xt4%d" % b), xt4[:, b, :], xh.ap()[(t + 1) * 128:(t + 2) * 128, :], writes=[Bxt4[b]])
                for dc in range(4):
                    for kc in range(16):
                        lt = axT[:, kc, t * 128:(t + 1) * 128] if kc < 8 else fxT[:, kc - 8, t * 128:(t + 1) * 128]
                        T.op(pe, lambda e: e.matmul(po[:, b, dc, :], lhsT=lt, rhs=wob[:, kc, dc * 512:(dc + 1) * 512], start=(kc == 0), stop=(kc == 15)),
                             reads=[BaxT, BfxT, Bwob], writes=[Bpo[b]])
                T.op(dve, lambda e: e.tensor_tensor(out=tm4[:, b, :], in0=po[:, b, :, :].rearrange("p a c -> p (a c)"), in1=g1[:], op=ALU.mult),
                     reads=[Bpo[b], Bg1], writes=[Btm4[b]])
                T.op(pool, lambda e: e.tensor_tensor(out=tm4[:, b, :], in0=tm4[:, b, :], in1=xt4[:, b, :], op=ALU.add), reads=[Bxt4[b]], writes=[Btm4[b]])
                T.dma(sp, T.dsem("tm4%d" % b), acc.ap()[t * 128:(t + 1) * 128, :], tm4[:, b, :], reads=[Btm4[b]], writes=[BaccT[t]])
            if debug:
                d = dbg_out("x1", [TOK, D])
                T.dma(sp, T.dsem("dbg2"), d.ap(), acc.ap(), reads=BaccT, writes=[Bdbg])
        esm.close()
        if stop_after <= 4:
            return finish()

        gTok = es0.enter_context(nc.sbuf_tensor("s_gTok", [128, NT, D], BF16))
        posm = es0.enter_context(nc.sbuf_tensor("s_posm", [128, NT, NE], F32))
        Bposm = Buf("posm")
        affA = es0.enter_context(nc.sbuf_tensor("s_affA", [128, NT, NE], F32))
        wgt = es0.enter_context(nc.sbuf_tensor("s_wgt", [128, NT, NE], F32))
        BgTok, BaffA, Bwgt = Buf("gTok", True), Buf("affA", True), Buf("wgt")
        Baffb, Baffg = Buf("affb"), Buf("affg")
        with Scope(T, ccx) as es5:
            a2 = es5.enter_context(nc.sbuf_tensor("s_a2", [128, D], F32))
            b2 = es5.enter_context(nc.sbuf_tensor("s_b2", [128, D], F32))
            tmb = es5.enter_context(nc.sbuf_tensor("s_tmb", [128, D], F32))
            x1t = es5.enter_context(nc.sbuf_tensor("s_x1t", [128, 2, D], F32))
            gT = es5.enter_context(nc.sbuf_tensor("s_gT", [128, 2, 16, 128], BF16))
            BgTt = [Buf("gT0"), Buf("gT1")]
            wrb = es5.enter_context(nc.sbuf_tensor("s_wrb", [128, 16, NE], BF16))
            st5 = es5.enter_context(nc.sbuf_tensor("s_st5", [128, NT, 8], F32))
            ex5 = es5.enter_context(nc.sbuf_tensor("s_ex5", [128, NE], F32))
            afT = es5.enter_context(nc.sbuf_tensor("s_afT", [NE, TOK], F32))
            ptg = es5.enter_context(nc.psum_tensor("p_tg", [128, 2, 1024], BF16))
            plg = es5.enter_context(nc.psum_tensor("p_lg", [128, NE], F32))
            paT = es5.enter_context(nc.psum_tensor("p_aT", [NE, 128], F32))
            Ba2, Bb2, Btmb, Bwrb, Bex5, BafT, Bptg, Bplg, BpaT = Buf("a2"), Buf("b2"), Buf("tmb"), Buf("wrb"), Buf("ex5"), Buf("afT", True), Buf("ptg"), Buf("plg"), Buf("paT")
            Bx1t = [Buf("x1t0"), Buf("x1t1")]
            T.op(pool, lambda e: e.memset(st5[:], 0.0), writes=[Bc])
            T.dma(pool, T.dsem("wrb"), wrb[:], wr.ap().rearrange("(kc p) n -> p kc n", p=128), writes=[Bwrb])
            T.dma(sp, T.dsem("a2"), a2[:].rearrange("p (r q) -> p r q", r=8), mod_bcast_ap(0, 4), reads=[Bmodg], writes=[Ba2])
            T.dma(sp, T.dsem("b2"), b2[:].rearrange("p (r q) -> p r q", r=8), mod_bcast_ap(0, 3), reads=[Bmodg], writes=[Bb2])
            T.dma(sp, T.dsem("tmb"), tmb[:], row_bcast(nffn), writes=[Btmb])
            T.op(dve, lambda e: e.scalar_tensor_tensor(out=a2[:], in0=a2[:], scalar=1.0, in1=tmb[:], op0=ALU.add, op1=ALU.mult), reads=[Btmb], writes=[Ba2])
            for t in range(NT):
                b = t % 2
                cols = slice(t * 128, (t + 1) * 128)
                T.dma(sp, T.dsem("x1t%d" % b), x1t[:, b, :], acc.ap()[t * 128:(t + 1) * 128, :], reads=[BaccT[t]], writes=[Bx1t[b]])
                Bst = Buf("st")
                sc = st5[:, t, :]
                rms_stats(sc, x1t[:, b, :], Bx1t[b], Bst, Btmb, tmb[:])
                T.op(dve, lambda e: e.scalar_tensor_tensor(out=tmb[:], in0=x1t[:, b, :], scalar=sc[:, 3:4], in1=a2[:], op0=ALU.mult, op1=ALU.mult),
                     reads=[Bx1t[b], Bst, Ba2], writes=[Btmb])
                T.op(pool, lambda e: e.tensor_tensor(out=gTok[:, t, :], in0=tmb[:], in1=b2[:], op=ALU.add), reads=[Btmb, Bb2], writes=[BgTok])
                for kc in range(16):
                    T.op(pe, lambda e: e.transpose(ptg[:, kc // 8, (kc % 8) * 128:(kc % 8 + 1) * 128], gTok[:, t, kc * 128:(kc + 1) * 128], ident[:]),
                         reads=[BgTok, Bc], writes=[Bptg])
                T.op(act, lambda e: e.activation(out=gT[:, b, 0:8, :], in_=ptg[:, 0, :].rearrange("p (a c) -> p a c", a=8), func=AF.Copy), reads=[Bptg], writes=[BgTt[b]])
                T.op(dve, lambda e: e.tensor_copy(out=gT[:, b, 8:16, :], in_=ptg[:, 1, :].rearrange("p (a c) -> p a c", a=8)), reads=[Bptg], writes=[BgTt[b]])
                for kc in range(16):
                    T.op(pe, lambda e: e.matmul(plg[:], lhsT=gT[:, b, kc, :], rhs=wrb[:, kc, :], start=(kc == 0), stop=(kc == 15)), reads=[BgTt[b], Bwrb], writes=[Bplg])
                T.op(dve, lambda e: e.tensor_reduce(out=sc[:, 4:5], in_=plg[:], axis=AX.X, op=ALU.max), reads=[Bplg], writes=[Bst])
                T.op(dve, lambda e: e.tensor_scalar(out=sc[:, 5:6], in0=sc[:, 4:5], scalar1=-1.0, scalar2=None, op0=ALU.mult), reads=[Bst], writes=[Bst])
                T.op(act, lambda e: e.activation(out=ex5[:], in_=plg[:], func=AF.Exp, bias=sc[:, 5:6], scale=1.0, accum_out=sc[:, 6:7]), reads=[Bplg, Bst], writes=[Bex5, Bst])
                T.op(dve, lambda e: e.reciprocal(out=sc[:, 7:8], in_=sc[:, 6:7]), reads=[Bst], writes=[Bst])
                T.op(dve, lambda e: e.tensor_scalar(out=affA[:, t, :], in0=ex5[:], scalar1=sc[:, 7:8], scalar2=None, op0=ALU.mult), reads=[Bex5, Bst], writes=[BaffA])
                T.op(pe, lambda e: e.matmul(paT[:], lhsT=affA[:, t, :], rhs=identf[:], start=True, stop=True), reads=[BaffA, Bc], writes=[BpaT])
                T.op(dve, lambda e: e.tensor_copy(out=afT[:, cols], in_=paT[:]), reads=[BpaT], writes=[BafT])
            T.dma(sp, T.dsem("afT"), affb.ap(), afT[:], reads=[BafT], writes=[Baffb])
            allgather(affb, affg, Baffb, Baffg)
            if debug:
                d = dbg_out("aff", [128, NT * NE])
                T.dma(sp, T.dsem("dbg2"), d.ap(), affA[:].rearrange("p a b -> p (a b)"), reads=[BaffA], writes=[Bdbg])
                d = dbg_out("affg", [128, TOK])
                T.dma(sp, T.dsem("dbg2"), d.ap(), affg.ap(), reads=[Baffg], writes=[Bdbg])
        if stop_after <= 5:
            return finish()

        NIT = 27
        with Scope(T, ccx) as es6:
            Tm = es6.enter_context(nc.sbuf_tensor("s_Tm", [128, TOK], F32))
            jk = es6.enter_context(nc.sbuf_tensor("s_jk", [128, TOK], BF16))
            bmt_ = es6.enter_context(nc.sbuf_tensor("s_bmat", [128, 128], F32))
            selt = es6.enter_context(nc.sbuf_tensor("s_sel", [128, NE], BF16))
            cn = es6.enter_context(nc.sbuf_tensor("s_cn", [128, 32], F32))
            sv = es6.enter_context(nc.sbuf_tensor("s_sv", [128, 8], F32))
            ptot = es6.enter_context(nc.psum_tensor("p_tot", [128, 1], F32))
            pmk = es6.enter_context(nc.psum_tensor("p_mk", [128, NT * NE], F32))
            BTm, Bjk, Bk6, Bcn, Bsv, Bptot, Bpmk = Buf("Tm"), Buf("jk"), Buf("k6", True), Buf("cn"), Buf("sv"), Buf("ptot"), Buf("pmk")
            T.dma(sp, T.dsem("Tm"), Tm[:], affg.ap(), reads=[Baffg], writes=[BTm])
            T.dma(sp, T.dsem("k6"), bmt_[:], bmati.ap(), writes=[Bk6])
            T.dma(sp, T.dsem("k6"), selt[:], seli.ap(), writes=[Bk6])
            T.op(pool, lambda e: e.memset(cn[:], 0.0), writes=[Bcn])
            T.op(pool, lambda e: e.memset(sv[:], 0.0), writes=[Bsv])
            T.op(pool, lambda e: e.memset(sv[:, 1:2], 1.5), writes=[Bsv])
            lo, hi, mid, cond, d1, d2 = [sv[:, i:i + 1] for i in range(6)]
            for it in range(NIT):
                T.op(dve, lambda e: e.tensor_tensor(out=mid, in0=lo, in1=hi, op=ALU.add), reads=[Bsv], writes=[Bsv])
                T.op(dve, lambda e: e.tensor_scalar(out=mid, in0=mid, scalar1=0.5, scalar2=None, op0=ALU.mult), reads=[Bsv], writes=[Bsv])
                T.op(dve, lambda e: e.tensor_scalar(out=jk[:], in0=Tm[:], scalar1=mid, scalar2=0.0, op0=ALU.is_ge, op1=ALU.add, accum_out=cn[:, it:it + 1]),
                     reads=[BTm, Bsv], writes=[Bjk, Bcn])
                T.op(pe, lambda e: e.matmul(ptot[:], lhsT=bmt_[:], rhs=cn[:, it:it + 1], start=True, stop=True), reads=[Bk6, Bcn], writes=[Bptot])
                T.op(dve, lambda e: e.tensor_scalar(out=cond, in0=ptot[:], scalar1=float(CAP) - 0.5, scalar2=None, op0=ALU.is_ge), reads=[Bptot], writes=[Bsv])
                T.op(dve, lambda e: e.tensor_tensor(out=d1, in0=mid, in1=lo, op=ALU.subtract), reads=[Bsv], writes=[Bsv])
                T.op(dve, lambda e: e.tensor_tensor(out=d2, in0=hi, in1=mid, op=ALU.subtract), reads=[Bsv], writes=[Bsv])
                T.op(dve, lambda e: e.scalar_tensor_tensor(out=lo, in0=d1, scalar=cond, in1=lo, op0=ALU.mult, op1=ALU.add), reads=[Bsv], writes=[Bsv])
                T.op(dve, lambda e: e.scalar_tensor_tensor(out=hi, in0=d2, scalar=cond, in1=mid, op0=ALU.mult, op1=ALU.add), reads=[Bsv], writes=[Bsv])
            T.op(dve, lambda e: e.tensor_scalar(out=jk[:], in0=Tm[:], scalar1=lo, scalar2=None, op0=ALU.is_ge), reads=[BTm, Bsv], writes=[Bjk])
            for t in range(NT):
                T.op(pe, lambda e: e.matmul(pmk[:, t * NE:(t + 1) * NE], lhsT=jk[:, t * 128:(t + 1) * 128], rhs=selt[:], start=True, stop=True), reads=[Bjk, Bk6], writes=[Bpmk])
            T.op(dve, lambda e: e.tensor_tensor(out=wgt[:].rearrange("p a b -> p (a b)"), in0=pmk[:], in1=affA[:].rearrange("p a b -> p (a b)"), op=ALU.mult),
                 reads=[Bpmk, BaffA], writes=[Bwgt])
            mkb = es6.enter_context(nc.sbuf_tensor("s_mkb", [128, NT, NE], BF16))
            ltri = es6.enter_context(nc.sbuf_tensor("s_ltri", [128, 128], BF16))
            ppos = es6.enter_context(nc.psum_tensor("p_pos", [128, NT * NE], F32))
            Bmkb, Bppos = Buf("mkb"), Buf("ppos")
            T.dma(sp, T.dsem("k6"), ltri[:], ltrii.ap(), writes=[Bk6])
            T.op(dve, lambda e: e.tensor_copy(out=mkb[:].rearrange("p a b -> p (a b)"), in_=pmk[:]), reads=[Bpmk], writes=[Bmkb])
            for t in range(NT):
                for t2 in range(t):
                    T.op(pe, lambda e: e.matmul(ppos[:, t * NE:(t + 1) * NE], lhsT=ones[:], rhs=mkb[:, t2, :], start=(t2 == 0), stop=False), reads=[Bmkb, Bc], writes=[Bppos])
                T.op(pe, lambda e: e.matmul(ppos[:, t * NE:(t + 1) * NE], lhsT=ltri[:], rhs=mkb[:, t, :], start=(t == 0), stop=True), reads=[Bmkb, Bk6], writes=[Bppos])
            pmf = posm[:].rearrange("p a b -> p (a b)")
            T.op(dve, lambda e: e.scalar_tensor_tensor(out=pmf, in0=ppos[:], scalar=1.0, in1=mkb[:].rearrange("p a b -> p (a b)"), op0=ALU.add, op1=ALU.mult), reads=[Bppos, Bmkb], writes=[Bposm])
            T.op(dve, lambda e: e.tensor_scalar(out=pmf, in0=pmf, scalar1=-1.0, scalar2=None, op0=ALU.add), reads=[Bposm], writes=[Bposm])
            if debug:
                d = dbg_out("posm", [128, NT * NE])
                T.dma(sp, T.dsem("dbg2"), d.ap(), pmf, reads=[Bposm], writes=[Bdbg])
                d = dbg_out("wgt", [128, NT * NE])
                T.dma(sp, T.dsem("dbg2"), d.ap(), wgt[:].rearrange("p a b -> p (a b)"), reads=[Bwgt], writes=[Bdbg])
                d = dbg_out("sv", [128, 8])
                T.dma(sp, T.dsem("dbg2"), d.ap(), sv[:], reads=[Bsv], writes=[Bdbg])
        if stop_after <= 6:
            return finish()

        with Scope(T, ccx) as es7:
            NS = 6
            ring = es7.enter_context(nc.sbuf_tensor("s_ring", [128, NS, 4096], BF16))
            iot = es7.enter_context(nc.sbuf_tensor("s_iot", [128, CAPL], F32))
            Pe = es7.enter_context(nc.sbuf_tensor("s_Pe", [128, 1, NT, CAPL], BF16))
            PTe = es7.enter_context(nc.sbuf_tensor("s_PTe", [128, 3, TOK], BF16))
            XsT = es7.enter_context(nc.sbuf_tensor("s_XsT", [128, 16, CAPL], BF16))
            HT = es7.enter_context(nc.sbuf_tensor("s_HT", [128, 8, CAPL], BF16))
            Yb = es7.enter_context(nc.sbuf_tensor("s_Yb", [128, 3, D], BF16))
            Yt = es7.enter_context(nc.sbuf_tensor("s_Yt", [128, 2, D], F32))
            g2 = es7.enter_context(nc.sbuf_tensor("s_g2", [128, D], F32))
            sg = es7.enter_context(nc.sbuf_tensor("s_sg", [128, 2, CAPL], F32))
            pA = es7.enter_context(nc.psum_tensor("p_A", [128, 2, 512], F32))
            pB = es7.enter_context(nc.psum_tensor("p_B", [128, 2, 512], F32))
            pO = es7.enter_context(nc.psum_tensor("p_O", [128, 4, 512], F32))
            Bring = [Buf("ring%d" % i) for i in range(NS)]
            Biot, BPTe, BXsT, BHT, BYb, Bg2, BpO = Buf("iot"), Buf("PTe", True), Buf("XsT", True), Buf("HT", True), Buf("Yb", True), Buf("g2"), Buf("pO")
            BPe = [Buf("Pe0", True), Buf("Pe1", True)]
            BYt = [Buf("Yt0"), Buf("Yt1")]
            Bsg = [Buf("sg0"), Buf("sg1")]
            BpA = [Buf("pA0"), Buf("pA1")]
            BpB = [Buf("pB0"), Buf("pB1")]
            T.dma(sp, T.dsem("g2"), g2[:].rearrange("p (r q) -> p r q", r=8), mod_bcast_ap(0, 5), reads=[Bmodg], writes=[Bg2])
            T.dma(sp, T.dsem("iot"), iot[:], iotai.ap(), writes=[Biot])
            pieces = []
            for e_ in range(NE):
                for j in range(4):
                    pieces.append(("g", e_, j))
                    pieces.append(("u", e_, j))
                for c_ in range(4):
                    pieces.append(("d", e_, c_))
            issued = [0]

            def ensure(upto):
                while issued[0] <= min(upto, len(pieces) - 1):
                    k = issued[0]
                    kind, e_, j = pieces[k]
                    sl = k % NS
                    if kind == "d":
                        src = wd.ap()[e_].rearrange("(fc p) d -> p fc d", p=128)[:, :, j * 512:(j + 1) * 512]
                        dst = ring[:, sl, :].rearrange("p (fc d) -> p fc d", fc=8)
                    else:
                        wsrc = wg if kind == "g" else wup
                        src = wsrc.ap()[e_].rearrange("(kc p) f -> p kc f", p=128)[:, :, j * 256:(j + 1) * 256]
                        dst = ring[:, sl, :].rearrange("p (kc f) -> p kc f", kc=16)
                    T.dma(pool, T.dsem("ring%d" % sl), dst, src, writes=[Bring[sl]])
                    issued[0] += 1

            def build_P(e_):
                pb_ = 0
                for t in range(NT):
                    T.op(dve if t % 2 == 0 else pool, lambda e: e.tensor_scalar(out=Pe[:, pb_, t, :], in0=iot[:], scalar1=posm[:, t, e_:e_ + 1], scalar2=None, op0=ALU.is_equal),
                         reads=[Biot, Bposm], writes=[BPe[pb_]])

            cA = 0
            cB = 0
            yc = 0
            ptv = pO[:].rearrange("p a c -> p (a c)").bitcast(BF16)
            ensure(5)
            build_P(0)
            for e_ in range(NE):
                base = e_ * 12
                pb_ = 0
                for dc in range(16):
                    a_ = cA % 2
                    cA += 1
                    for t in range(NT):
                        T.op(pe, lambda e: e.matmul(pA[:, a_, 0:CAPL], lhsT=gTok[:, t, dc * 128:(dc + 1) * 128], rhs=Pe[:, pb_, t, :], start=(t == 0), stop=(t == NT - 1)),
                             reads=[BgTok, BPe[pb_]], writes=[BpA[a_]])
                    T.op(act if dc % 2 == 0 else dve, (lambda e: e.activation(out=XsT[:, dc, :], in_=pA[:, a_, 0:CAPL], func=AF.Copy)) if dc % 2 == 0 else
                         (lambda e: e.tensor_copy(out=XsT[:, dc, :], in_=pA[:, a_, 0:CAPL])), reads=[BpA[a_]], writes=[BXsT])
                for s_ in range(3):
                    for h_ in range(2):
                        for q in range(8):
                            t = h_ * 8 + q
                            T.op(pe, lambda e: e.transpose(ptv[:, (h_ * 8 + q) * 128:(h_ * 8 + q + 1) * 128], Pe[:, pb_, t, s_ * 128:(s_ + 1) * 128], ident[:]),
                                 reads=[BPe[pb_], Bc], writes=[BpO])
                    T.op(act, lambda e: e.activation(out=PTe[:, s_, :], in_=ptv[:, 0:TOK], func=AF.Copy), reads=[BpO], writes=[BPTe])
                if e_ + 1 < NE:
                    build_P(e_ + 1)
                for j in range(4):
                    ensure(base + 2 * j + 5)
                    sg_, su_ = (base + 2 * j) % NS, (base + 2 * j + 1) % NS
                    wgp = ring[:, sg_, :].rearrange("p (kc f) -> p kc f", kc=16)
                    wupp = ring[:, su_, :].rearrange("p (kc f) -> p kc f", kc=16)
                    for fl in range(2):
                        fc = 2 * j + fl
                        a_ = cA % 2
                        cA += 1
                        b_ = cB % 2
                        cB += 1
                        for kc in range(16):
                            T.op(pe, lambda e: e.matmul(pA[:, a_, 0:CAPL], lhsT=wgp[:, kc, fl * 128:(fl + 1) * 128], rhs=XsT[:, kc, :], start=(kc == 0), stop=(kc == 15)),
                                 reads=[Bring[sg_], BXsT], writes=[BpA[a_]])
                        for kc in range(16):
                            T.op(pe, lambda e: e.matmul(pB[:, b_, 0:CAPL], lhsT=wupp[:, kc, fl * 128:(fl + 1) * 128], rhs=XsT[:, kc, :], start=(kc == 0), stop=(kc == 15)),
                                 reads=[Bring[su_], BXsT], writes=[BpB[b_]])
                        T.op(act, lambda e: e.activation(out=sg[:, a_, :], in_=pA[:, a_, 0:CAPL], func=AF.Silu), reads=[BpA[a_]], writes=[Bsg[a_]])
                        T.op(dve, lambda e: e.tensor_tensor(out=HT[:, fc, :], in0=pB[:, b_, 0:CAPL], in1=sg[:, a_, :], op=ALU.mult), reads=[BpB[b_], Bsg[a_]], writes=[BHT])
                ensure(base + 13)
                dsl = [(base + 8 + c_) % NS for c_ in range(4)]
                for s_ in range(3):
                    for dc in range(4):
                        b_ = cB % 2
                        cB += 1
                        wdp = ring[:, dsl[dc], :].rearrange("p (fc d) -> p fc d", fc=8)
                        for fc in range(8):
                            T.op(pe, lambda e: e.matmul(pB[:, b_, :], lhsT=HT[:, fc, s_ * 128:(s_ + 1) * 128], rhs=wdp[:, fc, :], start=(fc == 0), stop=(fc == 7)),
                                 reads=[BHT, Bring[dsl[dc]]], writes=[BpB[b_]])
                        T.op(act if dc % 2 == 0 else dve, (lambda e: e.activation(out=Yb[:, s_, dc * 512:(dc + 1) * 512], in_=pB[:, b_, :], func=AF.Copy)) if dc % 2 == 0 else
                             (lambda e: e.tensor_copy(out=Yb[:, s_, dc * 512:(dc + 1) * 512], in_=pB[:, b_, :])), reads=[BpB[b_]], writes=[BYb])
                for t in range(NT):
                    yb = yc % 2
                    yc += 1
                    for dc in range(4):
                        for s_ in range(3):
                            T.op(pe, lambda e: e.matmul(pO[:, dc, :], lhsT=PTe[:, s_, t * 128:(t + 1) * 128], rhs=Yb[:, s_, dc * 512:(dc + 1) * 512], start=(s_ == 0), stop=(s_ == 2)),
                                 reads=[BPTe, BYb], writes=[BpO])
                    T.op(dve, lambda e: e.scalar_tensor_tensor(out=Yt[:, yb, :], in0=pO[:].rearrange("p a c -> p (a c)"), scalar=wgt[:, t, e_:e_ + 1], in1=g2[:], op0=ALU.mult, op1=ALU.mult),
                         reads=[BpO, Bwgt, Bg2], writes=[BYt[yb]])
                    T.dma(pool, T.dsem("Yt%d" % yb), acc.ap()[t * 128:(t + 1) * 128, :], Yt[:, yb, :], reads=[BYt[yb]], writes=[BaccT[t]], accum_op=ALU.add)
                for bb in (BXsT, BHT, BYb, BPTe, BPe[pb_]):
                    bb.w = {}
        if stop_after <= 7:
            return finish()

        with Scope(T, ccx) as es8:
            gf = es8.enter_context(nc.sbuf_tensor("s_gf", [128, D], F32))
            x2t = es8.enter_context(nc.sbuf_tensor("s_x2t", [128, 2, D], F32))
            ot = es8.enter_context(nc.sbuf_tensor("s_ot", [128, 2, D], F32))
            tm8 = es8.enter_context(nc.sbuf_tensor("s_tm8", [128, D], F32))
            st8 = es8.enter_context(nc.sbuf_tensor("s_st8", [128, NT, 4], F32))
            Bgf, Btm8 = Buf("gf"), Buf("tm8")
            Bx2t = [Buf("x2t0"), Buf("x2t1")]
            Bot = [Buf("ot0"), Buf("ot1")]
            T.op(pool, lambda e: e.memset(st8[:], 0.0), writes=[Bc])
            T.dma(sp, T.dsem("gf"), gf[:], row_bcast(nfin), writes=[Bgf])
            for t in range(NT):
                b = t % 2
                T.dma(sp, T.dsem("x2t%d" % b), x2t[:, b, :], acc.ap()[t * 128:(t + 1) * 128, :], reads=[BaccT[t]], writes=[Bx2t[b]])
                Bst = Buf("st")
                sc = st8[:, t, :]
                rms_stats(sc, x2t[:, b, :], Bx2t[b], Bst, Btm8, tm8[:])
                T.op(dve, lambda e: e.scalar_tensor_tensor(out=ot[:, b, :], in0=x2t[:, b, :], scalar=sc[:, 3:4], in1=gf[:], op0=ALU.mult, op1=ALU.mult),
                     reads=[Bx2t[b], Bst, Bgf], writes=[Bot[b]])
                T.dma(sp, T.dsem("ot%d" % b), outd.ap()[t * 128:(t + 1) * 128, :], ot[:, b, :], reads=[Bot[b]], writes=[Bout])
        return finish()


def _consts(core):
    bf = ml_dtypes.bfloat16
    c = {}
    c["ident"] = np.eye(128, dtype=np.float32).astype(bf)
    c["identf"] = np.eye(128, dtype=np.float32)
    n = np.arange(core * TOK - 128, core * TOK + TOK + 128)
    row = (n // 64).astype(np.float64)
    col = (n % 64).astype(np.float64)
    freqs = (10000.0 ** (-np.arange(32, dtype=np.float32) / np.float32(32))).astype(np.float64)
    ar = row[:, None] * freqs[None, :]
    ac = col[:, None] * freqs[None, :]
    ang = np.concatenate([ar, ar, ac, ac], axis=-1)
    cos = np.cos(ang)
    sin = np.sin(ang)
    sgn = np.concatenate([-np.ones(32), np.ones(32), -np.ones(32), np.ones(32)])
    c["rope"] = np.concatenate([cos, sin * sgn[None, :]], axis=1).astype(np.float32)
    j = np.arange(128)[:, None]
    i = np.arange(128)[None, :]
    prev = (j >= i).astype(np.float32)
    nxt = (j <= i).astype(np.float32)
    m = np.zeros((128, 4, 128), np.float32)
    m[:, 0] = prev * (0.0 if core == 0 else 1.0)
    m[:, 1] = prev
    m[:, 2] = nxt * (0.0 if core == NCORES - 1 else 1.0)
    m[:, 3] = nxt
    c["masks"] = m.astype(bf)
    a = np.arange(128, dtype=np.float64)
    th = 2 * np.pi * np.outer(a, a) / 128.0
    c["r1"] = np.concatenate([np.cos(th), -np.sin(th)], axis=1).astype(np.float32).astype(bf)
    th2 = 2 * np.pi * np.outer(a, a) / float(SEQ)
    c["tw"] = np.stack([np.cos(th2), -np.sin(th2)], axis=1).astype(np.float32)
    c["c2s"] = np.stack([np.cos(th) / 128, np.sin(th) / 128, -np.sin(th) / 128], axis=1).astype(np.float32).astype(bf)
    cc = np.arange(256, dtype=np.float64)
    thc = 2 * np.pi * np.outer(cc, cc) / 256.0
    CS = np.stack([np.cos(thc) / 16, np.sin(thc) / 16], axis=0)
    ccs = CS.reshape(2, 2, 128, 256).transpose(2, 1, 0, 3)
    c["ccs"] = np.ascontiguousarray(ccs).astype(np.float32).astype(bf)
    p = np.arange(128)
    c["bmat"] = ((p[:, None] % 16) == (p[None, :] % 16)).astype(np.float32)
    sel = np.zeros((128, NE), np.float32)
    sel[core * 16 + np.arange(16), np.arange(16)] = 1.0
    c["sel"] = sel.astype(bf)
    c["iota"] = np.tile(np.arange(CAPL, dtype=np.float32)[None, :], (128, 1))
    c["ltri"] = (p[:, None] < p[None, :]).astype(np.float32).astype(bf)
    return c


def prep_inputs(x, c, ctx, c_ctx, w_mod, b_mod, norm_mix, w_in, sink, w_fourier, w_out,
                norm_ffn, w_router, w_gate, w_up, w_down, norm_final, ned=NE):
    f = lambda a: np.ascontiguousarray(np.asarray(a, dtype=np.float32))
    x2 = f(x)[0]
    xpad = np.concatenate([np.zeros((128, D), np.float32), x2, np.zeros((128, D), np.float32)], axis=0)
    cc = np.stack([f(c)[0], f(c_ctx)], axis=-1).reshape(16, 128, 2).transpose(1, 0, 2)
    wm = f(w_mod)[0].reshape(D, 6, 8, 256)
    bm = f(b_mod)[0].reshape(6, 8, 256)
    win = f(w_in)[0]
    shared = {
        "ctx": f(ctx)[0], "cc": np.ascontiguousarray(cc),
        "nmix": f(norm_mix).reshape(1, D), "nffn": f(norm_ffn).reshape(1, D), "nfin": f(norm_final).reshape(1, D),
        "wqkv": np.ascontiguousarray(win[:, :1536]), "sink": f(sink).reshape(1, 8),
        "wf": f(w_fourier)[0], "wout": f(w_out)[0], "wr": f(w_router)[0],
        "wg": f(w_gate)[0][:ned], "wup": f(w_up)[0][:ned], "wd": f(w_down)[0][:ned],
    }
    maps = []
    for r in range(NCORES):
        m = dict(shared)
        m["xh"] = np.ascontiguousarray(xpad[r * TOK:r * TOK + TOK + 256])
        m["wmod"] = np.ascontiguousarray(wm[:, :, r, :].reshape(D, 1536))
        m["bmod"] = np.ascontiguousarray(bm[:, r, :].reshape(1, 1536))
        m["wu"] = np.ascontiguousarray(win[:, 1536 + r * 128:1536 + (r + 1) * 128])
        m.update(_consts(r))
        maps.append(m)
    return maps


_NC_CACHE = {}


def kernel(**inputs):
    if "nc" not in _NC_CACHE:
        _NC_CACHE["nc"] = build()[0]
    nc = _NC_CACHE["nc"]
    maps = prep_inputs(**inputs)
    res = run_bass_kernel_spmd(nc, maps, core_ids=list(range(NCORES)))
    out = np.concatenate([np.asarray(res.results[r]["out"]) for r in range(NCORES)], axis=0)
    return out.reshape(1, SEQ, D).astype(np.float32)
```
